# Optimizing a Trainium2 kernel written in Bass

```python
import math
import jax, jax.numpy as jnp
from jax import lax
import numpy as np

D_MODEL = 2048
BATCH = 8
SEQ = 2048
DEPTH = 4

N_A_LAYERS = DEPTH // 2
N_B_LAYERS = DEPTH - N_A_LAYERS
N_DENSE_LAYERS = (DEPTH + 1) // 2
N_MOE_LAYERS = DEPTH // 2

CONV_WIDTH = 3

HEAD_DIM = 64
N_HEADS = D_MODEL // HEAD_DIM
N_KV_HEADS = N_HEADS // 8
GROUP = N_HEADS // N_KV_HEADS
KV_DIM = N_KV_HEADS * HEAD_DIM
WINDOW = 128
BLOCK = 128

D_FF = 5632
N_EXPERTS = 8
TOP_K = 2
D_FF_EXPERT = 7168

DEEPNORM_ALPHA = float((2 * DEPTH) ** 0.25)
DEEPNORM_BETA = float((8 * DEPTH) ** -0.25)
LN_EPS = 1e-5
NEG_INF = -1e30

kernel_name = "hybrid_shortconv_swa_sink_yoco_moe"


def layer_norm(x, g, b):
    xf = x.astype(jnp.float32)
    mu = jnp.mean(xf, axis=-1, keepdims=True)
    xc = xf - mu
    var = jnp.mean(xc * xc, axis=-1, keepdims=True)
    y = xc * lax.rsqrt(var + LN_EPS) * g.astype(jnp.float32) + b.astype(jnp.float32)
    return y.astype(x.dtype)


def swiglu(x, w_gate, w_up, w_down):
    return (jax.nn.silu(x @ w_gate) * (x @ w_up)) @ w_down


def short_conv_mixer(x, w_in, conv_w, w_out):
    proj = x @ w_in
    b_gate, c_gate, h = jnp.split(proj, 3, axis=-1)
    u = c_gate * h
    u_pad = jnp.pad(u, ((0, 0), (CONV_WIDTH - 1, 0), (0, 0)))
    seq = u.shape[1]
    conv = (conv_w[0] * u_pad[:, 0:seq]
            + conv_w[1] * u_pad[:, 1:seq + 1]
            + conv_w[2] * u_pad[:, 2:seq + 2])
    return (b_gate * conv) @ w_out


def sliding_window_gqa_sinks(x, k, v, w_q, sinks, w_o):
    bsz, seq, _ = x.shape
    nb = seq // BLOCK
    q = (x @ w_q).reshape(bsz, nb, BLOCK, N_KV_HEADS, GROUP, HEAD_DIM)

    def band(t):
        tp = jnp.pad(t, ((0, 0), (BLOCK, 0), (0, 0), (0, 0)))
        tp = tp.reshape(bsz, nb + 1, BLOCK, N_KV_HEADS, HEAD_DIM)
        return jnp.concatenate([tp[:, :-1], tp[:, 1:]], axis=2)

    kb, vb = band(k), band(v)
    scale = 1.0 / math.sqrt(HEAD_DIM)
    s = jnp.einsum('bnqkgd,bnskd->bnkgqs', q.astype(jnp.float32), kb.astype(jnp.float32)) * scale
    blk = jnp.arange(nb)[:, None, None] * BLOCK
    qpos = blk + jnp.arange(BLOCK)[None, :, None]
    kpos = blk - BLOCK + jnp.arange(2 * BLOCK)[None, None, :]
    mask = (kpos <= qpos) & (qpos - kpos < WINDOW) & (kpos >= 0)
    s = jnp.where(mask[None, :, None, None], s, NEG_INF)
    sink = jnp.broadcast_to(sinks.astype(jnp.float32).reshape(1, 1, N_KV_HEADS, GROUP, 1, 1),
                            s.shape[:-1] + (1,))
    p = jax.nn.softmax(jnp.concatenate([s, sink], axis=-1), axis=-1)[..., :-1]
    o = jnp.einsum('bnkgqs,bnskd->bnqkgd', p, vb.astype(jnp.float32))
    o = o.astype(x.dtype).reshape(bsz, seq, N_HEADS * HEAD_DIM)
    return o @ w_o


def moe_swiglu(x, w_router, w_gate, w_up, w_down):
    shape = x.shape
    xt = x.reshape(-1, shape[-1])
    logits = (xt @ w_router).astype(jnp.float32)
    top_val, top_idx = lax.top_k(logits, TOP_K)
    gates = jax.nn.softmax(top_val, axis=-1)
    combine = jnp.einsum('nk,nke->ne', gates,
                         jax.nn.one_hot(top_idx, N_EXPERTS, dtype=jnp.float32))
    out = jnp.zeros(xt.shape, jnp.float32)
    for e in range(N_EXPERTS):
        out = out + combine[:, e:e + 1] * swiglu(xt, w_gate[e], w_up[e], w_down[e]).astype(jnp.float32)
    return out.astype(x.dtype).reshape(shape)


def setup_inputs(seed: int = 0) -> dict:
    key = jax.random.key(seed)
    ks = jax.random.split(key, 20)
    f32 = jnp.float32
    d_in = D_MODEL ** -0.5
    nrm = lambda k, shape, s: jax.random.normal(k, shape, f32) * s
    return {
        "x": nrm(ks[0], (BATCH, SEQ, D_MODEL), 1.0),
        "conv_w_in": nrm(ks[1], (N_A_LAYERS, D_MODEL, 3 * D_MODEL), d_in),
        "conv_w": nrm(ks[2], (N_A_LAYERS, CONV_WIDTH, D_MODEL), CONV_WIDTH ** -0.5),
        "conv_w_out": nrm(ks[3], (N_A_LAYERS, D_MODEL, D_MODEL), d_in * DEEPNORM_BETA),
        "w_kv": nrm(ks[4], (D_MODEL, 2 * KV_DIM), d_in),
        "attn_w_q": nrm(ks[5], (N_B_LAYERS, D_MODEL, N_HEADS * HEAD_DIM), d_in),
        "attn_sinks": nrm(ks[6], (N_B_LAYERS, N_HEADS), 0.5),
        "attn_w_o": nrm(ks[7], (N_B_LAYERS, N_HEADS * HEAD_DIM, D_MODEL),
                        (N_HEADS * HEAD_DIM) ** -0.5 * DEEPNORM_BETA),
        "ln_g": 1.0 + nrm(ks[8], (DEPTH, 2, D_MODEL), 0.02),
        "ln_b": nrm(ks[9], (DEPTH, 2, D_MODEL), 0.02),
        "ffn_w_gate": nrm(ks[10], (N_DENSE_LAYERS, D_MODEL, D_FF), d_in),
        "ffn_w_up": nrm(ks[11], (N_DENSE_LAYERS, D_MODEL, D_FF), d_in),
        "ffn_w_down": nrm(ks[12], (N_DENSE_LAYERS, D_FF, D_MODEL), D_FF ** -0.5 * DEEPNORM_BETA),
        "moe_w_router": nrm(ks[13], (N_MOE_LAYERS, D_MODEL, N_EXPERTS), d_in),
        "moe_w_gate": nrm(ks[14], (N_MOE_LAYERS, N_EXPERTS, D_MODEL, D_FF_EXPERT), d_in),
        "moe_w_up": nrm(ks[15], (N_MOE_LAYERS, N_EXPERTS, D_MODEL, D_FF_EXPERT), d_in),
        "moe_w_down": nrm(ks[16], (N_MOE_LAYERS, N_EXPERTS, D_FF_EXPERT, D_MODEL),
                          D_FF_EXPERT ** -0.5 * DEEPNORM_BETA),
    }


def reference(x, conv_w_in, conv_w, conv_w_out, w_kv, attn_w_q, attn_sinks, attn_w_o,
              ln_g, ln_b, ffn_w_gate, ffn_w_up, ffn_w_down,
              moe_w_router, moe_w_gate, moe_w_up, moe_w_down):
    bsz, seq, _ = x.shape
    k_shared = v_shared = None
    for l in range(DEPTH):
        if l < N_A_LAYERS:
            mix = short_conv_mixer(x, conv_w_in[l], conv_w[l], conv_w_out[l])
        else:
            j = l - N_A_LAYERS
            mix = sliding_window_gqa_sinks(x, k_shared, v_shared,
                                           attn_w_q[j], attn_sinks[j], attn_w_o[j])
        x = layer_norm(DEEPNORM_ALPHA * x + mix, ln_g[l, 0], ln_b[l, 0])
        if l % 2 == 0:
            i = l // 2
            ff = swiglu(x, ffn_w_gate[i], ffn_w_up[i], ffn_w_down[i])
        else:
            i = l // 2
            ff = moe_swiglu(x, moe_w_router[i], moe_w_gate[i], moe_w_up[i], moe_w_down[i])
        x = layer_norm(DEEPNORM_ALPHA * x + ff, ln_g[l, 1], ln_b[l, 1])
        if l == N_A_LAYERS - 1:
            kv = x @ w_kv
            k_shared = kv[..., :KV_DIM].reshape(bsz, seq, N_KV_HEADS, HEAD_DIM)
            v_shared = kv[..., KV_DIM:].reshape(bsz, seq, N_KV_HEADS, HEAD_DIM)
    return x
```

```python
import contextlib
import numpy as np
import concourse.bass as bass
import concourse.mybir as mybir
from concourse.bass_utils import run_bass_kernel_spmd

F32 = mybir.dt.float32
BF16 = mybir.dt.bfloat16
AF = mybir.ActivationFunctionType
ALU = mybir.AluOpType
AX = mybir.AxisListType

P = 128
D = 2048
KD = 16
T = 512
NB = 4
HBC = 32
NSLOT = 4
ALPHA = float(8 ** 0.25)
EPS = 1e-5
SEM_LIMIT = 30000
NSEM = 20
DSPL = 4


class Prod:
    def __init__(self, b, name, step):
        self.b, self.name, self.step = b, name, step
        self.sem = None
        self.cnt = 0
        self.nsem = 0

    def new_event(self, ins):
        if self.sem is None or self.cnt + self.step > SEM_LIMIT:
            self.sem = self.b.new_sem(f"{self.name}{self.nsem}")
            self.nsem += 1
            self.cnt = 0
        self.cnt += self.step
        ins.then_inc(self.sem, self.step)
        return (self.sem, self.cnt, self.name)


class Waiter:
    def __init__(self, h, name):
        self.h, self.name = h, name
        self.waited = {}

    def wait(self, ev):
        sem, val, pname = ev
        if self.name == "pe" and pname == "pe":
            return
        k = id(sem)
        if self.waited.get(k, 0) >= val:
            return
        self.h.wait_ge(sem, val)
        self.waited[k] = val


class B:
    def __init__(self, S, DFF, DFE, NE, stop_after=None):
        self.S, self.DFF, self.DFE, self.NE = S, DFF, DFE, NE
        self.NT = S // T
        self.stop_after = stop_after
        self.nc = bass.Bass("TRN2", target_bir_lowering=False)
        self.es = contextlib.ExitStack()
        self.lastw = {}
        self.readers = {}
        self.rotc = {}
        self.psn = 0
        self.npiece = 0

    def new_sem(self, name):
        h = self.sem_pool[self.sem_used]
        self.sem_used += 1
        return h

    def sb(self, name, shape, dt):
        return self.es.enter_context(self.nc.sbuf_tensor(name, shape, dt))

    def dram_in(self, name, shape):
        return self.nc.dram_tensor(name, list(shape), F32, kind="ExternalInput").ap()

    def _pre(self, w, reads, writes):
        for k in reads:
            e = self.lastw.get(k)
            if e is not None:
                w.wait(e)
        for k in writes:
            e = self.lastw.get(k)
            if e is not None:
                w.wait(e)
            for r in self.readers.get(k, {}).values():
                w.wait(r)

    def _post(self, ev, reads, writes):
        for k in reads:
            self.readers.setdefault(k, {})[ev[2]] = ev
        for k in writes:
            self.lastw[k] = ev
            self.readers[k] = {}

    def op(self, w, prod, fn, reads=(), writes=()):
        self._pre(w, reads, writes)
        ins = fn()
        ev = prod.new_event(ins)
        self._post(ev, reads, writes)
        return ev

    def act(self, fn, reads=(), writes=()):
        return self.op(self.wact, self.pact, fn, reads, writes)

    def dve(self, fn, reads=(), writes=()):
        return self.op(self.wdve, self.pdve, fn, reads, writes)

    def pe(self, mms, reads=(), writes=()):
        self._pre(self.wpe, reads, writes)
        ins = None
        for m in mms:
            ins = self.nc.tensor.matmul(m[0], lhsT=m[1], rhs=m[2], start=m[3], stop=m[4])
        ev = self.ppe.new_event(ins)
        self._post(ev, reads, writes)
        return ev

    def dma(self, w, prod, out, in_, reads=(), writes=()):
        return self.op(w, prod, lambda: w.h.dma_start(out=out, in_=in_), reads, writes)

    def rot(self, name, n):
        i = self.rotc.get(name, 0)
        self.rotc[name] = i + 1
        return i % n

    def scr(self):
        i = self.rot("SCR", 8)
        return ("SCR", i), self.SCR[i]

    def ps(self):
        i = self.psn % 7
        self.psn += 1
        return i

    def piece(self, dmas):
        s = self.npiece % NSLOT
        self.npiece += 1
        key = ("WR", s)
        self._pre(self.wpool, (), (key,))
        ev = None
        for dst_fn, src in dmas:
            ins = self.nc.gpsimd.dma_start(out=dst_fn(self.WR[s]), in_=src)
            ev = self.pw[s].new_event(ins)
        self._post(ev, (), (key,))
        return s

    def wpiece(self, wmat, k0, kc, n0, n):
        v = wmat.rearrange("(c p) n -> p c n", p=P)
        return self.piece([((lambda W, a=a: W[:, a:min(a + DSPL, kc), 0:n]), v[:, k0 + a:k0 + min(a + DSPL, kc), n0:n0 + n])
                           for a in range(0, kc, DSPL)])

    def build(self):
        nc = self.nc
        S, DFF, DFE, NE = self.S, self.DFF, self.DFE, self.NE
        di = self.dram_in
        self.xT = di("xT", [D, S])
        self.conv_w_in = di("conv_w_in", [2, D, 3 * D])
        self.conv_w_out = di("conv_w_out", [2, D, D])
        self.w_kv = di("w_kv", [D, 512])
        self.attn_w_q = di("attn_w_q", [2, D, D])
        self.attn_w_o = di("attn_w_o", [2, D, D])
        self.ffn_w_gate = di("ffn_w_gate", [2, D, DFF])
        self.ffn_w_up = di("ffn_w_up", [2, D, DFF])
        self.ffn_w_down = di("ffn_w_down", [2, DFF, D])
        self.moe_w_gate = di("moe_w_gate", [2, NE, D, DFE])
        self.moe_w_up = di("moe_w_up", [2, NE, D, DFE])
        self.moe_w_down = di("moe_w_down", [2, NE, DFE, D])
        d_lng = di("lng", [P, 8 * 16])
        d_lnb = di("lnb", [P, 8 * 16])
        d_convw = di("convw", [P, 6 * 16])
        d_wrt = di("wrt", [P, 2 * 16 * NE])
        d_sinks = di("sinks", [P, 64])
        d_idf = di("idf", [P, P])
        d_maskc = di("maskc", [P, P])
        d_sel = di("sel", [NE, NE * P])
        self.outT = nc.dram_tensor("outT", [D, S], F32, kind="ExternalOutput").ap()

        sb = self.sb
        self.X = sb("X", [P, KD, T], F32)
        self.XB = sb("XB", [P, KD, T], BF16)
        self.HB = sb("HB", [P, HBC, T], BF16)
        self.WR = [sb(f"WR{i}", [P, 16, 512], BF16) for i in range(NSLOT)]
        self.SCR = [sb(f"SCR{i}", [P, T + 2], F32) for i in range(8)]
        self.MU = sb("MU", [P, T], F32)
        self.RS = sb("RS", [P, T], F32)
        self.NBI = sb("NBI", [P, T], F32)
        self.HALO = sb("HALO", [P, 2, KD, 2], F32)
        self.LNG = sb("LNG", [P, 8, 16], F32)
        self.LNB = sb("LNB", [P, 8, 16], F32)
        self.CONVW = sb("CONVW", [P, 6, 16], F32)
        self.WRT = sb("WRT", [P, 2, 16, NE], F32)
        self.SINKE = sb("SINKE", [P, 64], F32)
        self.IDF = sb("IDF", [P, P], F32)
        self.IDB = sb("IDB", [P, P], BF16)
        self.MASKF = sb("MASKF", [P, P], F32)
        self.MASKC = sb("MASKC", [P, P], BF16)
        self.MASKP = sb("MASKP", [P, P], BF16)
        self.ONESF = sb("ONESF", [P, P], F32)
        self.SEL = sb("SEL", [NE, NE, P], F32)
        self.LG = sb("LG", [P, NB, NE], F32)
        self.LG2 = sb("LG2", [P, NB, NE], F32)
        self.EQ1 = sb("EQ1", [P, NB, NE], F32)
        self.EQ2 = sb("EQ2", [P, NB, NE], F32)
        self.CMB = sb("CMB", [P, NB, NE], F32)
        self.M1 = sb("M1", [P, NB], F32)
        self.M2 = sb("M2", [P, NB], F32)
        self.G1 = sb("G1", [P, NB], F32)
        self.G2 = sb("G2", [P, NB], F32)
        self.CMBT = sb("CMBT", [NE, T], F32)
        self.CBC = [sb(f"CBC{i}", [P, T], F32) for i in range(2)]
        self.KTE = sb("KTE", [P, 4, T + P], BF16)
        self.KTO = sb("KTO", [P, 4, T + P], BF16)
        self.VA = sb("VA", [P, NB + 1, 4, 65], BF16)
        self.PT = [sb(f"PT{i}", [P, 1024], BF16) for i in range(2)]
        self.OT = [sb(f"OT{i}", [P, D], BF16) for i in range(2)]
        self.DEN = [sb(f"DEN{i}", [P, 4], F32) for i in range(2)]
        self.PS = [self.es.enter_context(nc.psum_tensor(f"PS{i}", [P, 512], F32)) for i in range(7)]

        self.sem_pool = [nc.alloc_semaphore(name=f"sm{i}") for i in range(NSEM)]
        self.sem_used = 0
        for h in self.sem_pool:
            nc.gpsimd.sem_clear(h)
        nc.all_engine_barrier()
        self.ppe = Prod(self, "pe", 1)
        self.pact = Prod(self, "act", 1)
        self.pdve = Prod(self, "dve", 1)
        self.pw = [Prod(self, f"w{i}_", 16) for i in range(NSLOT)]
        self.pld = Prod(self, "ld", 16)
        self.pst = Prod(self, "st", 16)
        self.wpe = Waiter(nc.tensor, "pe")
        self.wact = Waiter(nc.scalar, "act")
        self.wdve = Waiter(nc.vector, "dve")
        self.wpool = Waiter(nc.gpsimd, "pool")
        self.wsp = Waiter(nc.sync, "sp")

        sp, pld = self.wsp, self.pld
        cl = [
            (self.LNG[:].rearrange("p a b -> p (a b)"), d_lng, "LNG"),
            (self.LNB[:].rearrange("p a b -> p (a b)"), d_lnb, "LNB"),
            (self.CONVW[:].rearrange("p a b -> p (a b)"), d_convw, "CONVW"),
            (self.WRT[:].rearrange("p a b c -> p (a b c)"), d_wrt, "WRT"),
            (self.SINKE[:], d_sinks, "SINKE"),
            (self.IDF[:], d_idf, "IDF"),
            (self.MASKF[:], d_maskc, "MASKF"),
            (self.SEL[:].rearrange("p a b -> p (a b)"), d_sel, "SEL"),
        ]
        ev = None
        for o, i, k in cl:
            ev = self.dma(sp, pld, o, i, (), (k,))
        for o, i, k in cl:
            self.lastw[k] = ev
        self.act(lambda: nc.scalar.activation(out=self.SINKE[:], in_=self.SINKE[:], func=AF.Exp), ("SINKE",), ("SINKE",))
        self.dve(lambda: nc.vector.tensor_copy(out=self.IDB[:], in_=self.IDF[:]), ("IDF",), ("IDB",))
        self.dve(lambda: nc.vector.tensor_copy(out=self.MASKC[:], in_=self.MASKF[:]), ("MASKF",), ("MASKC",))
        self.dve(lambda: nc.vector.tensor_scalar(out=self.MASKP[:], in0=self.MASKF[:], scalar1=-1.0, scalar2=1.0,
                                                 op0=ALU.mult, op1=ALU.add), ("MASKF",), ("MASKP",))
        self.dve(lambda: nc.vector.memset(self.ONESF[:], 1.0 / D), (), ("ONESF",))
        self.dve(lambda: nc.vector.memset(self.HALO[:].rearrange("p a b c -> p (a b c)"), 0.0), (), ("HALO",))
        self.dve(lambda: nc.vector.memset(self.VA[:].rearrange("p a b c -> p (a b c)"), 1.0), (), ("VA",))
        self.dve(lambda: nc.vector.memset(self.KTE[:].rearrange("p a b -> p (a b)"), 0.0), (), ("KT2",))
        self.dve(lambda: nc.vector.memset(self.KTO[:].rearrange("p a b -> p (a b)"), 0.0), (), ("KT2",))
        ckeys = ["LNG", "LNB", "CONVW", "WRT", "SINKE", "IDF", "IDB", "MASKF", "MASKC", "MASKP", "ONESF", "SEL",
                 "HALO", "VA", "KT2"]
        for w in (self.wpe, self.wact, self.wdve):
            for k in ckeys:
                w.wait(self.lastw[k])
        for k in ckeys:
            self.lastw.pop(k, None)
            self.readers.pop(k, None)

        order = ["L0mix", "L0", "L1mix", "L1", "L2mix", "L2", "L3mix", "L3"]
        cut = None
        if self.stop_after in ("C0", "C1", "C2", "C3", "C4", "C5"):
            cut = self.stop_after
            nph = 0
        else:
            nph = len(order) if self.stop_after is None else order.index(self.stop_after) + 1

        for t in range(self.NT):
            self.load_tile(t)
            ph = 0
            if cut == "C1":
                self.conv_mixer(0, t, do_out=False)
            if cut == "C2":
                self.conv_mixer(0, t)
            if cut in ("C3", "C4", "C5"):
                self.layer_norm(0, 0, dbg=cut)
            for l in range(4):
                if ph >= nph:
                    break
                if l < 2:
                    self.conv_mixer(l, t)
                else:
                    self.attention(l - 2, t)
                self.layer_norm(l, 0)
                ph += 1
                if ph >= nph:
                    break
                if l % 2 == 0:
                    self.dense_ffn(l // 2)
                else:
                    self.moe_ffn(l // 2)
                last = (l == 3)
                self.layer_norm(l, 1, want_bf16=not last)
                ph += 1
                if l == 1 and ph < nph:
                    self.kv_proj(t)
            self.store_tile(t)
        for e in self.store_events:
            self.wsp.wait(e)
        self.wsp.h.wait_ge(self.pst.sem, self.pst.cnt)
        for pr in [self.ppe, self.pact, self.pdve, self.pld] + self.pw:
            if pr.sem is not None:
                self.wsp.h.wait_ge(pr.sem, pr.cnt)
        nc.all_engine_barrier()
        nc.clear_and_free_semaphores(self.sem_pool)
        nc.all_engine_barrier()
        self.es.close()
        return nc

    XK = [("X", c) for c in range(KD)]
    XBK = [("XB", c) for c in range(KD)]

    def load_tile(self, t):
        nc = self.nc
        src = self.xT.rearrange("(c p) s -> p c s", p=P)[:, :, t * T:(t + 1) * T]
        for a in range(0, KD, DSPL):
            ev = self.dma(self.wsp, self.pld, self.X[:, a:a + DSPL, :], src[:, a:a + DSPL, :], (), self.XK[a:a + DSPL])
        for k in self.XK:
            self.lastw[k] = ev
        for c in range(KD):
            if c % 2 == 0:
                self.dve(lambda: nc.vector.tensor_copy(out=self.XB[:, c, :], in_=self.X[:, c, :]), (("X", c),), (("XB", c),))
            else:
                self.act(lambda: nc.scalar.copy(out=self.XB[:, c, :], in_=self.X[:, c, :]), (("X", c),), (("XB", c),))

    store_events = None

    def store_tile(self, t):
        dst = self.outT.rearrange("(c p) s -> p c s", p=P)[:, :, t * T:(t + 1) * T]
        if self.store_events is None:
            self.store_events = []
        for a in range(0, KD, DSPL):
            ev = self.dma(self.wsp, self.pst, dst[:, a:a + DSPL, :], self.X[:, a:a + DSPL, :], self.XK[a:a + DSPL], ())
            self.store_events.append(ev)
        for k in self.XK:
            self.readers[k][ev[2]] = ev

    def mm_group(self, slot, mcol, kc, rhs_fn, rkeys, psb, start=True, stop=True, k_off=0):
        W = self.WR[slot]
        mms = []
        for k in range(kc):
            mms.append((self.PS[psb][:], W[:, k, mcol * P:(mcol + 1) * P], rhs_fn(k_off + k),
                        start and k == 0, stop and k == kc - 1))
        return self.pe(mms, [("WR", slot)] + rkeys, [("PS", psb)])

    def conv_mixer(self, l, t, do_out=True):
        nc = self.nc
        win = self.conv_w_in[l]
        for fb in range(4):
            sl = [self.wpiece(win, 0, KD, j * D + fb * 512, 512) for j in range(3)]
            for mm in range(4):
                m = fb * 4 + mm
                pb, pc, ph = self.ps(), self.ps(), self.ps()
                for slot, psb in zip(sl, (pb, pc, ph)):
                    self.mm_group(slot, mm, KD, lambda k: self.XB[:, k, :], self.XBK, psb)
                kb_, CBG = self.scr()
                kc_, CT = self.scr()
                ku_, U = self.scr()
                ka_, TA = self.scr()
                self.act(lambda: nc.scalar.copy(out=CBG[:, 0:T], in_=self.PS[pb][:]), [("PS", pb)], [kb_])
                self.act(lambda: nc.scalar.copy(out=CT[:, 0:T], in_=self.PS[pc][:]), [("PS", pc)], [kc_])
                hk = ("HALO", l, m)
                self.act(lambda: nc.scalar.copy(out=U[:, 0:2], in_=self.HALO[:, l, m, :]), [hk], [ku_])
                self.dve(lambda: nc.vector.tensor_tensor(out=U[:, 2:T + 2], in0=CT[:, 0:T], in1=self.PS[ph][:], op=ALU.mult),
                         [kc_, ("PS", ph), ku_], [ku_])
                self.act(lambda: nc.scalar.copy(out=self.HALO[:, l, m, :], in_=U[:, T:T + 2]), [ku_], [hk])
                cw = lambda j: self.CONVW[:, l * 3 + j, m:m + 1]
                self.dve(lambda: nc.vector.tensor_scalar(out=TA[:, 0:T], in0=U[:, 0:T], scalar1=cw(0), scalar2=None, op0=ALU.mult),
                         [ku_], [ka_])
                self.dve(lambda: nc.vector.scalar_tensor_tensor(out=TA[:, 0:T], in0=U[:, 1:T + 1], scalar=cw(1), in1=TA[:, 0:T],
                                                                op0=ALU.mult, op1=ALU.add), [ku_, ka_], [ka_])
                self.dve(lambda: nc.vector.scalar_tensor_tensor(out=TA[:, 0:T], in0=U[:, 2:T + 2], scalar=cw(2), in1=TA[:, 0:T],
                                                                op0=ALU.mult, op1=ALU.add), [ku_, ka_], [ka_])
                self.dve(lambda: nc.vector.tensor_tensor(out=self.HB[:, m, :], in0=TA[:, 0:T], in1=CBG[:, 0:T], op=ALU.mult),
                         [ka_, kb_], [("HB", m)])
        if do_out:
            self.out_proj(self.conv_w_out[l], 0)

    def out_proj(self, wmat, hb0):
        nc = self.nc
        hk = [("HB", hb0 + k) for k in range(KD)]
        for fb in range(4):
            slot = self.wpiece(wmat, 0, KD, fb * 512, 512)
            for mm in range(4):
                m = fb * 4 + mm
                pb = self.ps()
                self.mm_group(slot, mm, KD, lambda k: self.HB[:, hb0 + k, :], hk, pb)
                self.dve(lambda: nc.vector.scalar_tensor_tensor(out=self.X[:, m, :], in0=self.X[:, m, :], scalar=ALPHA,
                                                                in1=self.PS[pb][:], op0=ALU.mult, op1=ALU.add),
                         [("X", m), ("PS", pb)], [("X", m)])

    def layer_norm(self, l, j, want_bf16=True, dbg=None):
        nc = self.nc
        idx = l * 2 + j
        p_sum, p_sq = self.ps(), self.ps()
        for c in range(KD):
            kq_, SQ = self.scr()
            self.act(lambda: nc.scalar.activation(out=SQ[:, 0:T], in_=self.X[:, c, :], func=AF.Square), [("X", c)], [kq_])
            self.pe([(self.PS[p_sum][:], self.ONESF[:], self.X[:, c, :], c == 0, c == KD - 1)], [("X", c)], [("PS", p_sum)])
            self.pe([(self.PS[p_sq][:], self.ONESF[:], SQ[:, 0:T], c == 0, c == KD - 1)], [kq_], [("PS", p_sq)])
        MU, RS, NBI = self.MU, self.RS, self.NBI
        self.act(lambda: nc.scalar.copy(out=MU[:], in_=self.PS[p_sum][:]), [("PS", p_sum)], ["MU"])
        self.dve(lambda: nc.vector.tensor_tensor(out=NBI[:], in0=MU[:], in1=MU[:], op=ALU.mult), ["MU"], ["NBI"])
        self.dve(lambda: nc.vector.tensor_tensor(out=RS[:], in0=self.PS[p_sq][:], in1=NBI[:], op=ALU.subtract),
                 [("PS", p_sq), "NBI"], ["RS"])
        self.dve(lambda: nc.vector.tensor_scalar(out=RS[:], in0=RS[:], scalar1=EPS, scalar2=None, op0=ALU.add), ["RS"], ["RS"])
        if dbg == "C3":
            return
        self.act(lambda: nc.scalar.activation(out=RS[:], in_=RS[:], func=AF.Sqrt), ["RS"], ["RS"])
        self.dve(lambda: nc.vector.reciprocal(out=RS[:], in_=RS[:]), ["RS"], ["RS"])
        self.dve(lambda: nc.vector.scalar_tensor_tensor(out=NBI[:], in0=MU[:], scalar=-1.0, in1=RS[:], op0=ALU.mult, op1=ALU.mult),
                 ["MU", "RS"], ["NBI"])
        if dbg == "C4":
            return
        for c in range(KD):
            Xc = self.X[:, c, :]
            self.dve(lambda: nc.vector.tensor_tensor(out=Xc, in0=Xc, in1=RS[:], op=ALU.mult), [("X", c), "RS"], [("X", c)])
            self.dve(lambda: nc.vector.tensor_tensor(out=Xc, in0=Xc, in1=NBI[:], op=ALU.add), [("X", c), "NBI"], [("X", c)])
            if want_bf16:
                g, b = self.LNG[:, idx, c:c + 1], self.LNB[:, idx, c:c + 1]
                self.act(lambda: nc.scalar.activation(out=self.XB[:, c, :], in_=Xc, func=AF.Identity, scale=g, bias=b),
                         [("X", c)], [("XB", c)])
        for c in range(KD):
            Xc = self.X[:, c, :]
            g, b = self.LNG[:, idx, c:c + 1], self.LNB[:, idx, c:c + 1]
            self.dve(lambda: nc.vector.tensor_scalar(out=Xc, in0=Xc, scalar1=g, scalar2=b, op0=ALU.mult, op1=ALU.add),
                     [("X", c)], [("X", c)])

    def ffn_core(self, wg, wu, wd, F, cbc_key=None, cbc=None, first_scaled=True):
        nc = self.nc
        FC = F // P
        ng = -(-FC // HBC)
        per = -(-FC // (ng * 4)) * 4
        groups = []
        c0 = 0
        while c0 < FC:
            n = min(per, FC - c0)
            groups.append((c0, n))
            c0 += n
        first = first_scaled
        for (c0, n) in groups:
            for jb in range(n // 4):
                col = (c0 + jb * 4) * P
                sg = self.wpiece(wg, 0, KD, col, 512)
                su = self.wpiece(wu, 0, KD, col, 512)
                for mm in range(4):
                    hc = jb * 4 + mm
                    pg, pu = self.ps(), self.ps()
                    self.mm_group(sg, mm, KD, lambda k: self.XB[:, k, :], self.XBK, pg)
                    self.mm_group(su, mm, KD, lambda k: self.XB[:, k, :], self.XBK, pu)
                    ks_, SGf = self.scr()
                    SG = SGf[:, 0:T]
                    self.act(lambda: nc.scalar.activation(out=SG, in_=self.PS[pg][:], func=AF.Silu), [("PS", pg)], [ks_])
                    self.dve(lambda: nc.vector.tensor_tensor(out=self.HB[:, hc, :], in0=SG, in1=self.PS[pu][:], op=ALU.mult),
                             [ks_, ("PS", pu)], [("HB", hc)])
            npp = -(-n // 16)
            kper = -(-n // npp)
            kparts = []
            k0 = 0
            while k0 < n:
                kc = min(kper, n - k0)
                kparts.append((k0, kc))
                k0 += kc
            for db in range(4):
                slots = [self.wpiece(wd, c0 + k0, kc, db * 512, 512) for (k0, kc) in kparts]
                for mm in range(4):
                    m = db * 4 + mm
                    pb = self.ps()
                    for pi, ((k0, kc), slot) in enumerate(zip(kparts, slots)):
                        self.mm_group(slot, mm, kc, lambda k: self.HB[:, k, :], [("HB", k0 + kk) for kk in range(kc)], pb,
                                      start=(pi == 0), stop=(pi == len(kparts) - 1), k_off=k0)
                    if cbc is None:
                        if first:
                            self.dve(lambda: nc.vector.scalar_tensor_tensor(out=self.X[:, m, :], in0=self.X[:, m, :], scalar=ALPHA,
                                                                            in1=self.PS[pb][:], op0=ALU.mult, op1=ALU.add),
                                     [("X", m), ("PS", pb)], [("X", m)])
                        else:
                            self.dve(lambda: nc.vector.tensor_tensor(out=self.X[:, m, :], in0=self.X[:, m, :], in1=self.PS[pb][:], op=ALU.add),
                                     [("X", m), ("PS", pb)], [("X", m)])
                    else:
                        ka_, TAf = self.scr()
                        TA = TAf[:, 0:T]
                        self.dve(lambda: nc.vector.tensor_tensor(out=TA, in0=self.PS[pb][:], in1=cbc[:], op=ALU.mult),
                                 [("PS", pb), cbc_key], [ka_])
                        if first:
                            self.dve(lambda: nc.vector.scalar_tensor_tensor(out=self.X[:, m, :], in0=self.X[:, m, :], scalar=ALPHA,
                                                                            in1=TA, op0=ALU.mult, op1=ALU.add),
                                     [("X", m), ka_], [("X", m)])
                        else:
                            self.dve(lambda: nc.vector.tensor_tensor(out=self.X[:, m, :], in0=self.X[:, m, :], in1=TA, op=ALU.add),
                                     [("X", m), ka_], [("X", m)])
            first = False

    def dense_ffn(self, i):
        self.ffn_core(self.ffn_w_gate[i], self.ffn_w_up[i], self.ffn_w_down[i], self.DFF)

    def moe_ffn(self, i):
        nc = self.nc
        NE = self.NE
        pl = self.ps()
        for s in range(NB):
            mms = [(self.PS[pl][:, s * NE:(s + 1) * NE], self.X[:, c, s * P:(s + 1) * P], self.WRT[:, i, c, :], c == 0, c == KD - 1)
                   for c in range(KD)]
            self.pe(mms, self.XK, [("PS", pl)])
        LG, LG2, EQ1, EQ2, CMB = self.LG, self.LG2, self.EQ1, self.EQ2, self.CMB
        M1, M2, G1, G2 = self.M1, self.M2, self.G1, self.G2
        v3 = lambda a: a[:]
        bc = lambda a: a[:].unsqueeze(2).broadcast_to([P, NB, NE])
        self.act(lambda: nc.scalar.copy(out=LG[:].rearrange("p a b -> p (a b)"), in_=self.PS[pl][:, 0:NB * NE]), [("PS", pl)], ["LG"])
        self.dve(lambda: nc.vector.reduce_max(out=M1[:], in_=v3(LG), axis=AX.X), ["LG"], ["M1"])
        self.dve(lambda: nc.vector.tensor_tensor(out=v3(EQ1), in0=v3(LG), in1=bc(M1), op=ALU.is_equal), ["LG", "M1"], ["EQ1"])
        self.dve(lambda: nc.vector.scalar_tensor_tensor(out=v3(LG2), in0=v3(EQ1), scalar=-1e30, in1=v3(LG), op0=ALU.mult, op1=ALU.add),
                 ["EQ1", "LG"], ["LG2"])
        self.dve(lambda: nc.vector.reduce_max(out=M2[:], in_=v3(LG2), axis=AX.X), ["LG2"], ["M2"])
        self.dve(lambda: nc.vector.tensor_tensor(out=v3(EQ2), in0=v3(LG2), in1=bc(M2), op=ALU.is_equal), ["LG2", "M2"], ["EQ2"])
        self.dve(lambda: nc.vector.tensor_tensor(out=G2[:], in0=M2[:], in1=M1[:], op=ALU.subtract), ["M1", "M2"], ["G2"])
        self.act(lambda: nc.scalar.activation(out=G2[:], in_=G2[:], func=AF.Exp), ["G2"], ["G2"])
        self.dve(lambda: nc.vector.tensor_scalar(out=G2[:], in0=G2[:], scalar1=1.0, scalar2=None, op0=ALU.add), ["G2"], ["G2"])
        self.dve(lambda: nc.vector.reciprocal(out=G1[:], in_=G2[:]), ["G2"], ["G1"])
        self.dve(lambda: nc.vector.tensor_scalar(out=G2[:], in0=G1[:], scalar1=-1.0, scalar2=1.0, op0=ALU.mult, op1=ALU.add), ["G1"], ["G2"])
        self.dve(lambda: nc.vector.tensor_tensor(out=v3(EQ1), in0=v3(EQ1), in1=bc(G1), op=ALU.mult), ["EQ1", "G1"], ["EQ1"])
        self.dve(lambda: nc.vector.tensor_tensor(out=v3(EQ2), in0=v3(EQ2), in1=bc(G2), op=ALU.mult), ["EQ2", "G2"], ["EQ2"])
        self.dve(lambda: nc.vector.tensor_tensor(out=v3(CMB), in0=v3(EQ1), in1=v3(EQ2), op=ALU.add), ["EQ1", "EQ2"], ["CMB"])
        pt = self.ps()
        for s in range(NB):
            self.pe([(self.PS[pt][0:NE, s * P:(s + 1) * P], CMB[:, s, :], self.IDF[:], True, True)], ["CMB"], [("PS", pt)])
        self.act(lambda: nc.scalar.copy(out=self.CMBT[:], in_=self.PS[pt][0:NE, :]), [("PS", pt)], ["CMBT"])
        for e in range(NE):
            pc = self.ps()
            self.pe([(self.PS[pc][:], self.SEL[:, e, :], self.CMBT[:], True, True)], ["CMBT"], [("PS", pc)])
            ic = self.rot("CBC", 2)
            self.act(lambda: nc.scalar.copy(out=self.CBC[ic][:], in_=self.PS[pc][:]), [("PS", pc)], [("CBC", ic)])
            self.ffn_core(self.moe_w_gate[i, e], self.moe_w_up[i, e], self.moe_w_down[i, e], self.DFE,
                          cbc_key=("CBC", ic), cbc=self.CBC[ic], first_scaled=(e == 0))

    def kv_proj(self, t):
        nc = self.nc
        KTE, KTO, VA = self.KTE, self.KTO, self.VA
        if t > 0:
            self.act(lambda: nc.scalar.copy(out=KTE[:, :, 0:P], in_=KTE[:, :, T:T + P]), ["KT2"], ["KT2"])
            self.act(lambda: nc.scalar.copy(out=KTO[:, :, 0:P], in_=KTO[:, :, T:T + P]), ["KT2"], ["KT2"])
            self.dve(lambda: nc.vector.tensor_copy(out=VA[:, 0, :, :], in_=VA[:, NB, :, :]), ["VA"], ["VA"])
        wk = self.w_kv.rearrange("(c p) n -> p c n", p=P)
        dm = []
        for g in range(4):
            for h in range(2):
                for a in range(0, KD, DSPL):
                    dm.append(((lambda W, g=g, h=h, a=a: W[:, a:a + DSPL, g * P + h * 64: g * P + h * 64 + 64]),
                               wk[:, a:a + DSPL, g * 64:(g + 1) * 64]))
        sk = self.piece(dm)
        sv = self.wpiece(self.w_kv, 0, KD, 256, 256)
        for g in range(4):
            pb = self.ps()
            self.mm_group(sk, g, KD, lambda k: self.XB[:, k, :], self.XBK, pb)
            self.act(lambda: nc.scalar.copy(out=KTE[0:64, g, P:P + T], in_=self.PS[pb][0:64, :]), [("PS", pb)], ["KT2"])
            self.act(lambda: nc.scalar.copy(out=KTO[64:128, g, P:P + T], in_=self.PS[pb][64:128, :]), [("PS", pb)], ["KT2"])
        Wv = self.WR[sv]
        for s in range(NB):
            pb = self.ps()
            mms = [(self.PS[pb][:, 0:256], self.XB[:, k, s * P:(s + 1) * P], Wv[:, k, 0:256], k == 0, k == KD - 1) for k in range(KD)]
            self.pe(mms, [("WR", sv)] + self.XBK, [("PS", pb)])
            self.dve(lambda: nc.vector.tensor_copy(out=VA[:, 1 + s, :, 0:64],
                                                   in_=self.PS[pb][:, 0:256].rearrange("p (g d) -> p g d", g=4)),
                     [("PS", pb)], ["VA"])

    def attention(self, j, t):
        nc = self.nc
        KTE, KTO, VA, HB = self.KTE, self.KTO, self.VA, self.HB
        wq = self.attn_w_q[j]
        for g in range(4):
            slot = self.wpiece(wq, 0, KD, g * 512, 512)
            for cc in range(4):
                c = g * 4 + cc
                pb = self.ps()
                self.mm_group(slot, cc, KD, lambda k: self.XB[:, k, :], self.XBK, pb)
                self.act(lambda: nc.scalar.copy(out=HB[:, c, :], in_=self.PS[pb][:]), [("PS", pb)], [("HB", c)])
        for n in range(NB):
            io = self.rot("OT", 2)
            OT = self.OT[io]
            kbs = [1] if (t == 0 and n == 0) else [0, 1]
            for g in range(4):
                for kb in kbs:
                    ip = self.rot("PT", 2)
                    PT = self.PT[ip]
                    kcol = (n + kb) * P
                    for half in range(2):
                        pb = self.ps()
                        mms = []
                        for h4 in range(4):
                            hi = half * 4 + h4
                            cc = g * 4 + hi // 2
                            KTx = KTE if hi % 2 == 0 else KTO
                            mms.append((self.PS[pb][:, h4 * P:(h4 + 1) * P], KTx[:, g, kcol:kcol + P],
                                        HB[:, cc, n * P:(n + 1) * P], True, True))
                        self.pe(mms, ["KT2"] + [("HB", g * 4 + half * 2 + q) for q in range(2)], [("PS", pb)])
                        kf_, PTFf = self.scr()
                        PTF = PTFf[:, 0:512]
                        self.act(lambda: nc.scalar.activation(out=PTF, in_=self.PS[pb][:], func=AF.Exp, scale=0.125),
                                 [("PS", pb)], [kf_])
                        mask = self.MASKC if kb == 1 else self.MASKP
                        self.dve(lambda: nc.vector.tensor_tensor(out=PT[:, half * 512:(half + 1) * 512].rearrange("p (h q) -> p h q", h=4),
                                                                 in0=PTF.rearrange("p (h q) -> p h q", h=4),
                                                                 in1=mask[:].unsqueeze(1).broadcast_to([P, 4, P]), op=ALU.mult),
                                 [kf_], [("PT", ip, half)])
                    kb_last = kb
                pts = [(self.rotc["PT"] - len(kbs) + q) % 2 for q in range(len(kbs))]
                for half in range(2):
                    pb = self.ps()
                    mms = []
                    for h4 in range(4):
                        hi = half * 4 + h4
                        for q, kb in enumerate(kbs):
                            mms.append((self.PS[pb][:, h4 * P:h4 * P + 65], self.PT[pts[q]][:, hi * P:(hi + 1) * P],
                                        VA[:, n + kb, g, :], q == 0, q == len(kbs) - 1))
                    self.pe(mms, ["VA"] + [("PT", pts[q], half) for q in range(len(kbs))], [("PS", pb)])
                    idn = self.rot("DEN", 2)
                    DEN = self.DEN[idn]
                    pv = self.PS[pb][:].rearrange("p (h q) -> p h q", h=4)
                    s0 = j * 32 + g * 8 + half * 4
                    self.dve(lambda: nc.vector.tensor_tensor(out=DEN[:], in0=pv[:, :, 64], in1=self.SINKE[:, s0:s0 + 4], op=ALU.add),
                             [("PS", pb)], [("DEN", idn)])
                    self.dve(lambda: nc.vector.reciprocal(out=DEN[:], in_=DEN[:]), [("DEN", idn)], [("DEN", idn)])
                    o0 = (g * 8 + half * 4) * 64
                    self.dve(lambda: nc.vector.tensor_tensor(out=OT[:, o0:o0 + 256].rearrange("p (h d) -> p h d", h=4),
                                                             in0=pv[:, :, 0:64], in1=DEN[:].unsqueeze(2).broadcast_to([P, 4, 64]),
                                                             op=ALU.mult),
                             [("PS", pb), ("DEN", idn)], [("OT", io)])
            for q4 in range(4):
                pb = self.ps()
                mms = [(self.PS[pb][:, cc * P:(cc + 1) * P], OT[:, (q4 * 4 + cc) * P:(q4 * 4 + cc + 1) * P], self.IDB[:], True, True)
                       for cc in range(4)]
                self.pe(mms, [("OT", io)], [("PS", pb)])
                self.act(lambda: nc.scalar.copy(out=HB[:, 16 + q4 * 4:16 + q4 * 4 + 4, n * P:(n + 1) * P],
                                                in_=self.PS[pb][:].rearrange("p (c q) -> p c q", c=4)),
                         [("PS", pb)], [("HB", 16 + q4 * 4 + cc) for cc in range(4)])
        self.out_proj(self.attn_w_o[j], 16)


_CACHE = {}


def get_program(S, DFF, DFE, NE, stop_after=None):
    key = (S, DFF, DFE, NE, stop_after)
    if key not in _CACHE:
        _CACHE[key] = B(S, DFF, DFE, NE, stop_after).build()
    return _CACHE[key]


def make_in_maps(inputs, NE):
    x = np.asarray(inputs["x"], dtype=np.float32)
    bsz = x.shape[0]
    f = lambda k: np.ascontiguousarray(np.asarray(inputs[k], dtype=np.float32))
    lng = np.ascontiguousarray(f("ln_g").reshape(8, 16, P).transpose(2, 0, 1).reshape(P, 128))
    lnb = np.ascontiguousarray(f("ln_b").reshape(8, 16, P).transpose(2, 0, 1).reshape(P, 128))
    convw = np.ascontiguousarray(f("conv_w").reshape(6, 16, P).transpose(2, 0, 1).reshape(P, 96))
    wrt = np.ascontiguousarray(f("moe_w_router").reshape(2, 16, P, NE).transpose(2, 0, 1, 3).reshape(P, 2 * 16 * NE))
    sinks = np.ascontiguousarray(np.broadcast_to(f("attn_sinks").reshape(1, 64), (P, 64)))
    idf = np.eye(P, dtype=np.float32)
    maskc = (np.arange(P)[:, None] <= np.arange(P)[None, :]).astype(np.float32)
    sel = np.zeros((NE, NE, P), np.float32)
    for e in range(NE):
        sel[e, e, :] = 1.0
    sel = sel.reshape(NE, NE * P)
    shared = {k: f(k) for k in ["conv_w_in", "conv_w_out", "w_kv", "attn_w_q", "attn_w_o", "ffn_w_gate", "ffn_w_up",
                                "ffn_w_down", "moe_w_gate", "moe_w_up", "moe_w_down"]}
    shared.update(lng=lng, lnb=lnb, convw=convw, wrt=wrt, sinks=sinks, idf=idf, maskc=maskc, sel=sel)
    maps = []
    for b in range(bsz):
        m = dict(shared)
        m["xT"] = np.ascontiguousarray(x[b].T)
        maps.append(m)
    return maps


def run(inputs, stop_after=None, trace=False):
    x = np.asarray(inputs["x"])
    bsz, S, _ = x.shape
    DFF = inputs["ffn_w_gate"].shape[2]
    NE = inputs["moe_w_gate"].shape[1]
    DFE = inputs["moe_w_gate"].shape[3]
    nc = get_program(S, DFF, DFE, NE, stop_after)
    maps = make_in_maps(inputs, NE)
    res = run_bass_kernel_spmd(nc, maps, core_ids=list(range(bsz)), trace=trace)
    out = np.stack([np.ascontiguousarray(r["outT"].T) for r in res.results], axis=0)
    return out.astype(np.float32), res


def kernel(**inputs):
    out, _ = run(inputs)
    return out
```
